# Optimizing a Trainium2 kernel written in Bass

```python
import math
import jax, jax.numpy as jnp
from jax import lax
import numpy as np

D_MODEL = 2048
BATCH = 1
SEQ = 16384
DEPTH = 1

CHUNK = 64
SSD_HEAD_DIM = 64
SSD_INNER = D_MODEL
SSD_HEADS = SSD_INNER // SSD_HEAD_DIM
SSD_GROUPS = 8
SSD_HEADS_PER_GROUP = SSD_HEADS // SSD_GROUPS
SSD_STATE = 128
CONV_WIDTH = 4
XBC_DIM = SSD_INNER + 2 * SSD_GROUPS * SSD_STATE
S5_WIDTH = D_MODEL // 2
S5_GROUP_CH = 16
S5_GROUPS = S5_WIDTH // S5_GROUP_CH
S5_STATE = 64
N_EXPERT_GROUPS = 4
EXPERTS_PER_GROUP = 8
N_EXPERTS = N_EXPERT_GROUPS * EXPERTS_PER_GROUP
TOP_K_INNER = 2
EXPERT_FF = D_MODEL // 4
MOE_BLOCK = 128
IN_SIZES = (SSD_INNER, XBC_DIM, SSD_HEADS, S5_WIDTH, 2 * D_MODEL)
IN_DIM = sum(IN_SIZES)
IN_SPLITS = tuple(int(s) for s in np.cumsum(IN_SIZES)[:-1])
RMS_EPS = 1e-6

kernel_name = "hybrid_ssd_s5_hmoe_block"


def rmsnorm(v, w):
    vf = v.astype(jnp.float32)
    vf = vf * lax.rsqrt(jnp.mean(vf * vf, axis=-1, keepdims=True) + RMS_EPS)
    return vf.astype(v.dtype) * w


def causal_depthwise_conv(v, w, bias):
    seq = v.shape[1]
    vp = jnp.pad(v, ((0, 0), (CONV_WIDTH - 1, 0), (0, 0)))
    out = bias
    for k in range(CONV_WIDTH):
        out = out + vp[:, k:k + seq] * w[k]
    return out


def ssd_chunked(xh, dt, a_log, bmat, cmat, d_skip):
    b, seq = xh.shape[0], xh.shape[1]
    nc = seq // CHUNK
    G, R, P, N = SSD_GROUPS, SSD_HEADS_PER_GROUP, SSD_HEAD_DIM, SSD_STATE
    a = -jnp.exp(a_log.astype(jnp.float32))
    a_dt = (dt * a).reshape(b, nc, CHUNK, G, R).transpose(0, 3, 4, 1, 2)
    xdt = (xh * dt[..., None]).reshape(b, nc, CHUNK, G, R, P)
    bc = bmat.reshape(b, nc, CHUNK, G, N)
    cc = cmat.reshape(b, nc, CHUNK, G, N)
    a_cs = jnp.cumsum(a_dt, axis=-1)
    causal = jnp.tril(jnp.ones((CHUNK, CHUNK), dtype=bool))
    seg = a_cs[..., :, None] - a_cs[..., None, :]
    decay_in = jnp.where(causal, jnp.exp(jnp.where(causal, seg, 0.0)), 0.0)
    cb = jnp.einsum('bclgn,bcsgn->bgcls', cc, bc)
    y_diag = jnp.einsum('bgcls,bgrcls,bcsgrp->bclgrp', cb, decay_in, xdt)
    decay_states = jnp.exp(a_cs[..., -1:] - a_cs)
    states = jnp.einsum('bclgn,bgrcl,bclgrp->bcgrpn', bc, decay_states, xdt)
    chunk_decay = jnp.exp(a_cs[..., -1])

    def step(carry, inp):
        st, dec = inp
        return carry * dec[..., None, None] + st, carry

    init = jnp.zeros((b, G, R, P, N), states.dtype)
    _, prev = lax.scan(step, init, (jnp.moveaxis(states, 1, 0), jnp.moveaxis(chunk_decay, -1, 0)))
    prev = jnp.moveaxis(prev, 0, 1)
    y_off = jnp.einsum('bclgn,bcgrpn,bgrcl->bclgrp', cc, prev, jnp.exp(a_cs))
    y = (y_diag + y_off).reshape(b, seq, SSD_HEADS, P) + xh * d_skip[:, None]
    return y.reshape(b, seq, SSD_HEADS * P)


def s5_combine(e1, e2):
    a1r, a1i, b1r, b1i = e1
    a2r, a2i, b2r, b2i = e2
    return (a2r * a1r - a2i * a1i,
            a2r * a1i + a2i * a1r,
            a2r * b1r - a2i * b1i + b2r,
            a2r * b1i + a2i * b1r + b2i)


def s5_mixer(u, lam_re, lam_im, log_dt, b_re, b_im, c_re, c_im, d_skip):
    b, seq, w = u.shape
    ug = u.reshape(b, seq, S5_GROUPS, S5_GROUP_CH)
    f32 = jnp.float32
    lr, li = lam_re.astype(f32), lam_im.astype(f32)
    dt = jnp.exp(log_dt.astype(f32))[:, None]
    mag = jnp.exp(lr * dt)
    ang = li * dt
    abar_r, abar_i = mag * jnp.cos(ang), mag * jnp.sin(ang)
    den = lr * lr + li * li
    nr, ni = abar_r - 1.0, abar_i
    coef_r = (nr * lr + ni * li) / den
    coef_i = (ni * lr - nr * li) / den
    bre, bim = b_re.astype(f32), b_im.astype(f32)
    bb_r = coef_r[..., None] * bre - coef_i[..., None] * bim
    bb_i = coef_r[..., None] * bim + coef_i[..., None] * bre
    bu_r = jnp.einsum('blgc,gnc->blgn', ug, bb_r)
    bu_i = jnp.einsum('blgc,gnc->blgn', ug, bb_i)
    a_r = jnp.broadcast_to(abar_r, bu_r.shape)
    a_i = jnp.broadcast_to(abar_i, bu_i.shape)
    _, _, s_r, s_i = lax.associative_scan(s5_combine, (a_r, a_i, bu_r, bu_i), axis=1)
    y = (jnp.einsum('blgn,gcn->blgc', s_r, c_re.astype(f32))
         - jnp.einsum('blgn,gcn->blgc', s_i, c_im.astype(f32)))
    return y.reshape(b, seq, w) + d_skip * u


def hierarchical_moe(h, w_rg, b_rg, w_re, b_re, w_g, w_u, w_d):
    b, seq, d = h.shape
    T = b * seq
    ht = h.reshape(T, d)
    f32 = jnp.float32
    logit_g = (ht @ w_rg).astype(f32) + b_rg.astype(f32)
    p_g = jax.nn.softmax(logit_g, axis=-1)
    grp = jnp.argmax(logit_g, axis=-1).astype(jnp.int32)
    pg_sel = jnp.take_along_axis(p_g, grp[:, None], axis=-1)
    logit_e = ((ht @ w_re).astype(f32) + b_re.astype(f32)).reshape(T, N_EXPERT_GROUPS, EXPERTS_PER_GROUP)
    logit_e = jnp.take_along_axis(logit_e, grp[:, None, None], axis=1)[:, 0]
    top_v, top_i = lax.top_k(logit_e, TOP_K_INNER)
    w_sel = jax.nn.softmax(top_v, axis=-1) * pg_sel
    expert_ids = (grp[:, None] * EXPERTS_PER_GROUP + top_i).reshape(-1).astype(jnp.int32)
    weights = w_sel.reshape(-1)
    token_idx = jnp.repeat(jnp.arange(T, dtype=jnp.int32), TOP_K_INNER)
    n_assign = T * TOP_K_INNER
    order = jnp.argsort(expert_ids)
    e_sorted = expert_ids[order]
    counts = jnp.bincount(expert_ids, length=N_EXPERTS)
    padded = ((counts + MOE_BLOCK - 1) // MOE_BLOCK) * MOE_BLOCK
    start_sorted = jnp.cumsum(counts) - counts
    ends_padded = jnp.cumsum(padded)
    start_padded = ends_padded - padded
    rank = jnp.arange(n_assign, dtype=jnp.int32) - start_sorted[e_sorted]
    dest = start_padded[e_sorted] + rank
    n_rows = n_assign + N_EXPERTS * MOE_BLOCK
    n_blocks = n_rows // MOE_BLOCK
    row_token = jnp.full((n_rows,), T, jnp.int32).at[dest].set(token_idx[order])
    row_weight = jnp.zeros((n_rows,), weights.dtype).at[dest].set(weights[order])
    block_start = jnp.arange(n_blocks, dtype=jnp.int32) * MOE_BLOCK
    block_expert = jnp.clip(jnp.searchsorted(ends_padded, block_start, side='right'), 0, N_EXPERTS - 1)
    ht_pad = jnp.concatenate([ht, jnp.zeros((1, d), ht.dtype)], axis=0)

    def run_block(args):
        tok, wgt, e = args
        xb = ht_pad[tok]
        y = (jax.nn.silu(xb @ w_g[e]) * (xb @ w_u[e])) @ w_d[e]
        return y * wgt[:, None].astype(y.dtype)

    ys = lax.map(run_block, (row_token.reshape(n_blocks, MOE_BLOCK),
                             row_weight.reshape(n_blocks, MOE_BLOCK), block_expert))
    out = jnp.zeros((T + 1, d), ys.dtype).at[row_token].add(ys.reshape(n_rows, d))[:T]
    return out.reshape(b, seq, d).astype(h.dtype)


def setup_inputs(seed: int = 0) -> dict:
    key = jax.random.key(seed)
    ks = jax.random.split(key, 32)
    f32 = jnp.float32
    L = DEPTH

    def nrm(k, shape, scale):
        return jax.random.normal(k, shape, f32) * scale

    def gain(k, shape):
        return 1.0 + 0.02 * jax.random.normal(k, shape, f32)

    dt0 = jnp.exp(jax.random.uniform(ks[5], (L, SSD_HEADS), f32, math.log(1e-3), math.log(1e-1)))
    return {
        "x": nrm(ks[0], (BATCH, SEQ, D_MODEL), 1.0),
        "norm_mix_w": gain(ks[1], (L, D_MODEL)),
        "w_in": nrm(ks[2], (L, D_MODEL, IN_DIM), D_MODEL ** -0.5),
        "conv_w": nrm(ks[3], (L, CONV_WIDTH, XBC_DIM), CONV_WIDTH ** -0.5),
        "conv_b": nrm(ks[4], (L, XBC_DIM), 0.02),
        "dt_bias": dt0 + jnp.log(-jnp.expm1(-dt0)),
        "a_log": jnp.log(jax.random.uniform(ks[6], (L, SSD_HEADS), f32, 1.0, 16.0)),
        "d_ssd": gain(ks[7], (L, SSD_HEADS)),
        "norm_ssd_w": gain(ks[8], (L, SSD_INNER)),
        "w_a_up": nrm(ks[9], (L, SSD_INNER, D_MODEL), SSD_INNER ** -0.5),
        "s5_lambda_re": -0.5 + nrm(ks[10], (L, S5_GROUPS, S5_STATE), 0.01),
        "s5_lambda_im": math.pi * jnp.arange(S5_STATE, dtype=f32) + nrm(ks[11], (L, S5_GROUPS, S5_STATE), 0.01),
        "s5_log_dt": jax.random.uniform(ks[12], (L, S5_GROUPS), f32, math.log(1e-3), math.log(1e-1)),
        "s5_b_re": nrm(ks[13], (L, S5_GROUPS, S5_STATE, S5_GROUP_CH), (2.0 * S5_GROUP_CH) ** -0.5),
        "s5_b_im": nrm(ks[14], (L, S5_GROUPS, S5_STATE, S5_GROUP_CH), (2.0 * S5_GROUP_CH) ** -0.5),
        "s5_c_re": nrm(ks[15], (L, S5_GROUPS, S5_GROUP_CH, S5_STATE), (2.0 * S5_STATE) ** -0.5),
        "s5_c_im": nrm(ks[16], (L, S5_GROUPS, S5_GROUP_CH, S5_STATE), (2.0 * S5_STATE) ** -0.5),
        "s5_d": nrm(ks[17], (L, S5_WIDTH), 1.0),
        "w_glu": nrm(ks[18], (L, S5_WIDTH, S5_WIDTH), S5_WIDTH ** -0.5),
        "w_b_up": nrm(ks[19], (L, S5_WIDTH, D_MODEL), S5_WIDTH ** -0.5),
        "gate_b": nrm(ks[20], (L, 2 * D_MODEL), 0.02),
        "w_out": nrm(ks[21], (L, D_MODEL, D_MODEL), D_MODEL ** -0.5),
        "norm_ffn_w": gain(ks[22], (L, D_MODEL)),
        "w_route_group": nrm(ks[23], (L, D_MODEL, N_EXPERT_GROUPS), D_MODEL ** -0.5),
        "b_route_group": nrm(ks[24], (L, N_EXPERT_GROUPS), 0.01),
        "w_route_expert": nrm(ks[25], (L, D_MODEL, N_EXPERTS), D_MODEL ** -0.5),
        "b_route_expert": nrm(ks[26], (L, N_EXPERTS), 0.01),
        "w_exp_gate": nrm(ks[27], (L, N_EXPERTS, D_MODEL, EXPERT_FF), D_MODEL ** -0.5),
        "w_exp_up": nrm(ks[28], (L, N_EXPERTS, D_MODEL, EXPERT_FF), D_MODEL ** -0.5),
        "w_exp_down": nrm(ks[29], (L, N_EXPERTS, EXPERT_FF, D_MODEL), EXPERT_FF ** -0.5),
        "norm_final_w": gain(ks[30], (D_MODEL,)),
    }


def reference(x, norm_mix_w, w_in, conv_w, conv_b, dt_bias, a_log, d_ssd, norm_ssd_w, w_a_up,
              s5_lambda_re, s5_lambda_im, s5_log_dt, s5_b_re, s5_b_im, s5_c_re, s5_c_im, s5_d,
              w_glu, w_b_up, gate_b, w_out, norm_ffn_w, w_route_group, b_route_group,
              w_route_expert, b_route_expert, w_exp_gate, w_exp_up, w_exp_down, norm_final_w):
    b, seq, _ = x.shape
    for i in range(DEPTH):
        h = rmsnorm(x, norm_mix_w[i])
        proj = h @ w_in[i]
        z, xbc, dt_raw, u, gate_logits = jnp.split(proj, IN_SPLITS, axis=-1)
        xbc = jax.nn.silu(causal_depthwise_conv(xbc, conv_w[i], conv_b[i]))
        xs, bm, cm = jnp.split(xbc, (SSD_INNER, SSD_INNER + SSD_GROUPS * SSD_STATE), axis=-1)
        dt = jax.nn.softplus(dt_raw.astype(jnp.float32) + dt_bias[i].astype(jnp.float32))
        ya = ssd_chunked(xs.reshape(b, seq, SSD_HEADS, SSD_HEAD_DIM), dt, a_log[i],
                         bm.reshape(b, seq, SSD_GROUPS, SSD_STATE),
                         cm.reshape(b, seq, SSD_GROUPS, SSD_STATE), d_ssd[i])
        ya = rmsnorm(ya.astype(x.dtype) * jax.nn.silu(z), norm_ssd_w[i]) @ w_a_up[i]
        yb = s5_mixer(u, s5_lambda_re[i], s5_lambda_im[i], s5_log_dt[i], s5_b_re[i], s5_b_im[i],
                      s5_c_re[i], s5_c_im[i], s5_d[i]).astype(x.dtype)
        v = jax.nn.gelu(yb)
        yb = (v * jax.nn.sigmoid(v @ w_glu[i])) @ w_b_up[i]
        g = jax.nn.sigmoid(gate_logits + gate_b[i])
        merged = g[..., :D_MODEL] * ya + g[..., D_MODEL:] * yb
        x = x + merged @ w_out[i]
        h2 = rmsnorm(x, norm_ffn_w[i])
        x = x + hierarchical_moe(h2, w_route_group[i], b_route_group[i], w_route_expert[i],
                                 b_route_expert[i], w_exp_gate[i], w_exp_up[i], w_exp_down[i])
    return rmsnorm(x, norm_final_w)
```

```python
import contextlib
import math
import numpy as np
import concourse.bass as bass
import concourse.mybir as mybir
from concourse.bass_utils import run_bass_kernel_spmd

F32 = mybir.dt.float32
BF16 = mybir.dt.bfloat16
I32 = mybir.dt.int32
AF = mybir.ActivationFunctionType
ALU = mybir.AluOpType
AX = mybir.AxisListType

NCORES = 8
TOK = 2048
NT = 16
D = 2048
CAP = 256
NSLOT = 32 * CAP
EPS = 1e-6
XW = 2048 + 32 + 64
CC_INC = 1


class Prog:
    CE = ('pe', 'act', 'dve', 'pool')

    def __init__(self, nc, ndma=14):
        self.nc = nc
        self.engobj = {'pe': nc.tensor, 'act': nc.scalar, 'dve': nc.vector,
                       'pool': nc.gpsimd, 'sp': nc.sync}
        self.q = {e: [] for e in self.engobj}
        self.cnt = {e: 0 for e in self.CE}
        self.csem = {e: nc.alloc_semaphore('c_' + e) for e in self.CE}
        self.dsem, self.dval, self.drr = {}, {}, {}
        for qn in ('sp', 'pool', 'bg'):
            self.dsem[qn] = [nc.alloc_semaphore(f'd_{qn}{i}') for i in range(ndma)]
            self.dval[qn] = [0] * ndma
            self.drr[qn] = 0
        self.seen = {e: {} for e in self.engobj}
        self.lastw = {}
        self.readers = {}

    def _sem(self, key):
        if key[0] == 'c':
            return self.csem[key[1]]
        return self.dsem[key[1]][key[2]]

    def _deps(self, eng, reads, writes):
        need = {}

        def add(tok):
            if tok is None:
                return
            k, v = tok
            if eng == 'pe' and k == ('c', 'pe'):
                return
            if need.get(k, 0) < v:
                need[k] = v
        for r in reads:
            add(self.lastw.get(r))
        for w in writes:
            add(self.lastw.get(w))
            for t in self.readers.get(w, ()):
                add(t)
        out = []
        for k, v in need.items():
            if self.seen[eng].get(k, 0) < v:
                self.seen[eng][k] = v
                out.append((self._sem(k), v))
        return out

    def _record(self, tok, reads, writes):
        for w in writes:
            self.lastw[w] = tok
            self.readers[w] = []
        for r in reads:
            self.readers.setdefault(r, []).append(tok)

    def op(self, eng, fn, reads=(), writes=()):
        ex = [r for r in reads if r.startswith(('bank', 'PA', 'PB'))]
        if ex:
            writes = list(writes) + ex
        waits = self._deps(eng, reads, writes)
        self.cnt[eng] += 1
        tok = (('c', eng), self.cnt[eng])
        sem = self.csem[eng]

        def run(e, waits=waits, fn=fn, sem=sem):
            for s, v in waits:
                e.wait_ge(s, v)
            fn(e).then_inc(sem, 1)
        self.q[eng].append(run)
        self._record(tok, reads, writes)
        return tok

    def dma(self, qn, fn, reads=(), writes=(), bg=False):
        eng_q = qn
        waits = self._deps(qn, reads, writes)
        if bg:
            qn = 'bg'
        i = self.drr[qn]
        self.drr[qn] = (i + 1) % len(self.dsem[qn])
        key = ('d', qn, i)
        prev = self.dval[qn][i]
        if prev > 0 and self.seen[eng_q].get(key, 0) < prev:
            self.seen[eng_q][key] = prev
            waits.append((self.dsem[qn][i], prev))
        self.dval[qn][i] = prev + 16
        tok = (key, prev + 16)
        sem = self.dsem[qn][i]

        def run(e, waits=waits, fn=fn, sem=sem):
            for s, v in waits:
                e.wait_ge(s, v)
            fn(e).then_inc(sem, 16)
        self.q[eng_q].append(run)
        self._record(tok, reads, writes)
        return tok

    def cc(self, fn, reads=(), writes=()):
        if not hasattr(self, 'ccsem'):
            self.ccsem = self.nc.alloc_semaphore('cc_sem')
            self.ccval = 0
            self.dsem['cc'] = [self.ccsem]
            self.dval['cc'] = [0]
        waits = self._deps('pool', reads, writes)
        inc = CC_INC
        self.dval['cc'][0] += inc
        tok = (('d', 'cc', 0), self.dval['cc'][0])
        sem = self.ccsem

        def run(e, waits=waits, fn=fn, sem=sem):
            for s, v in waits:
                e.wait_ge(s, v)
            if CC_INC == 1:
                fn(e).then_inc(sem)
            else:
                fn(e).then_inc(sem, CC_INC)
        self.q['pool'].append(run)
        self._record(tok, reads, writes)
        return tok

    def barrier(self, include_bg=False):
        toks = [(('c', e), self.cnt[e]) for e in self.CE if self.cnt[e] > 0]
        for qn in self.dsem:
            if qn == 'bg' and not include_bg:
                continue
            for i, v in enumerate(self.dval[qn]):
                if v > 0:
                    toks.append((('d', qn, i), v))
        for eng in self.engobj:
            waits = []
            for k, v in toks:
                if self.seen[eng].get(k, 0) < v:
                    self.seen[eng][k] = v
                    waits.append((self._sem(k), v))

            def run(e, waits=waits):
                for s, v in waits:
                    e.wait_ge(s, v)
            self.q[eng].append(run)
        self.lastw = {}
        self.readers = {}

    def finish(self):
        self.barrier(include_bg=True)
        with self.nc.Block() as block:
            @block.sync
            def _(e):
                for f in self.q['sp']:
                    f(e)

            @block.tensor
            def _(e):
                for f in self.q['pe']:
                    f(e)

            @block.scalar
            def _(e):
                for f in self.q['act']:
                    f(e)

            @block.vector
            def _(e):
                for f in self.q['dve']:
                    f(e)

            @block.gpsimd
            def _(e):
                for f in self.q['pool']:
                    f(e)


def ktl(w):
    K, N = w.shape
    return np.ascontiguousarray(w.reshape(K // 128, 128, N).transpose(1, 0, 2))


def col(v):
    return np.ascontiguousarray(v.reshape(-1, 128).T)


def rep(v):
    return np.ascontiguousarray(np.broadcast_to(v.reshape(1, -1), (128, v.size)))


INPUT_SPECS = {}


def build(stop_after=None, dbg=None, ncores=NCORES):
    nc = bass.Bass("TRN2", target_bir_lowering=False)
    P = Prog(nc)
    din = {}

    def IN(name, shape, dt=F32):
        din[name] = nc.dram_tensor(name, list(shape), dt, kind="ExternalInput").ap()
        return din[name]

    x_c = IN("x_c", [TOK, D]); x_h = IN("x_h", [128, D])
    w_z = IN("w_z", [128, 16, 2048]); w_xbc = IN("w_xbc", [128, 16, 4096]); w_dt = IN("w_dt", [128, 16, 32])
    w_u = IN("w_u", [128, 16, 1024]); w_g = IN("w_g", [128, 16, 4096])
    nwmix = IN("nwmix", [128, 16]); cw = IN("cw", [128, 32, 4]); cb = IN("cb", [128, 32])
    dtb = IN("dtb", [128, 32]); alog = IN("alog", [128, 32]); dssd = IN("dssd", [128, 32])
    nssd = IN("nssd", [128, 16]); gateb = IN("gateb", [128, 32])
    w_a = IN("w_a", [128, 16, 2048]); w_o = IN("w_o", [128, 16, 2048])
    lre = IN("lre", [128, 64]); lim = IN("lim", [128, 64]); ldt = IN("ldt", [128, 64])
    b1 = IN("b1", [128, 64, 16]); b2 = IN("b2", [128, 64, 16]); c1 = IN("c1", [128, 64, 16])
    d5 = IN("d5", [128, 8]); w_glu = IN("w_glu", [128, 8, 1024]); w_b = IN("w_b", [128, 8, 2048])
    nffn = IN("nffn", [128, 2048]); nfin = IN("nfin", [128, 2048])
    w_r = IN("w_r", [128, 16, 36]); b_r = IN("b_r", [128, 36])
    w_eg = IN("w_eg", [32, 128, 16, 512]); w_eu = IN("w_eu", [32, 128, 16, 512]); w_ed = IN("w_ed", [32, 128, 4, 2048])
    c_id = IN("c_id", [128, 128]); c_ut = IN("c_ut", [128, 128]); c_gt = IN("c_gt", [128, 128])
    c_sut = IN("c_sut", [128, 128]); c_psw = IN("c_psw", [128, 128]); c_sign = IN("c_sign", [128, 2])
    c_mcol = IN("c_mcol", [128, 8]); c_ecap = IN("c_ecap", [128, 32]); c_tokid = IN("c_tokid", [128, 16], I32)
    c_sinit = IN("c_sinit", [128, 66], I32); c_mk = IN("c_mk", [128, 8])
    out = nc.dram_tensor("out", [TOK, D], F32, kind="ExternalOutput").ap()
    dbg_t = {}
    if dbg:
        for k, shp in dbg.items():
            if shp is None:
                continue
            dbg_t[k] = nc.dram_tensor("dbg_" + k, list(shp), F32, kind="ExternalOutput").ap()

    def SCR(name, shape, dt):
        kind = "ExternalOutput" if (dbg and name in dbg) else "Internal"
        return nc.dram_tensor(name, list(shape), dt, kind=kind).ap()
    zs_d = SCR("zs_d", [TOK, 2048], BF16)
    gs_d = SCR("gs_d", [4096, TOK], BF16)
    xc_d = SCR("xc_d", [4096, TOK], BF16)
    xs_d = SCR("xs_d", [TOK, 2048], BF16)
    btm_d = SCR("btm_d", [TOK, 1024], BF16)
    u_d = SCR("u_d", [1024, TOK], BF16)
    ybg_d = SCR("ybg_d", [2048, TOK], BF16)
    x1_d = SCR("x1_d", [TOK, D], F32)
    h2_d = SCR("h2_d", [TOK + 128, D], BF16)
    yall_d = SCR("yall_d", [NSLOT + 128, D], BF16)
    slot_d = SCR("slot_d", [NSLOT + 128, 1], I32)
    y5_d = SCR("y5_d", [1024, TOK], F32)
    weg_d = SCR("weg_d", [32, 128 * 16, 512], BF16)
    weu_d = SCR("weu_d", [32, 128 * 16, 512], BF16)
    wed_d = SCR("wed_d", [32, 128 * 4, 2048], BF16)
    yn_d = SCR("yn_d", [2048, TOK], BF16)
    xch_src = SCR("xch_src", [128, XW], F32)
    xch_dst = SCR("xch_dst", [ncores * 128, XW], F32)

    PA = nc.alloc_psum_tensor("PA", [128, 2048], F32)
    PB = nc.alloc_psum_tensor("PB", [128, 2048], F32)

    def bank(i):
        t = PA if i < 4 else PB
        j = i % 4
        return t[:, j * 512:(j + 1) * 512]

    def mm(o, lhsT, rhs, st, sp, R, W):
        P.op('pe', lambda e: e.matmul(o, lhsT=lhsT, rhs=rhs, start=st, stop=sp), R, W)

    def tr(o, i, idn, R, W):
        P.op('pe', lambda e: e.transpose(out=o, in_=i, identity=idn), R, W)

    def tt(eng, o, a, b, op, R, W):
        P.op(eng, lambda e: e.tensor_tensor(out=o, in0=a, in1=b, op=op), R, W)

    def ts(eng, o, a, s1, s2, op0, op1, R, W):
        if op1 is None:
            P.op(eng, lambda e: e.tensor_scalar(out=o, in0=a, scalar1=s1, scalar2=None, op0=op0), R, W)
        else:
            P.op(eng, lambda e: e.tensor_scalar(out=o, in0=a, scalar1=s1, scalar2=s2, op0=op0, op1=op1), R, W)

    def stt(o, a, s, b, op0, op1, R, W, eng='dve'):
        P.op(eng, lambda e: e.scalar_tensor_tensor(out=o, in0=a, scalar=s, in1=b, op0=op0, op1=op1), R, W)

    def act(o, i, f, R, W, scale=None, accum=None, bias=None):
        kw = {}
        if scale is not None:
            kw['scale'] = scale
        if accum is not None:
            kw['accum_out'] = accum
        if bias is not None:
            kw['bias'] = bias
        P.op('act', lambda e: e.activation(out=o, in_=i, func=f, **kw), R, W)

    def cp(eng, o, i, R, W):
        if eng == 'act':
            P.op('act', lambda e: e.activation(out=o, in_=i, func=AF.Copy), R, W)
        else:
            P.op(eng, lambda e: e.tensor_copy(out=o, in_=i), R, W)

    def ld(q, o, i, R, W):
        P.dma(q, lambda e: e.dma_start(out=o, in_=i), R, W)

    def memset(eng, o, val, W):
        P.op(eng, lambda e: e.memset(o, val), (), W)

    def rstd_from_ss(ssx, R, W):
        ts('dve', ssx[:, 1:2], ssx[:, 0:1], 1.0 / D, EPS, ALU.mult, ALU.add, R, [W + '1'])
        act(ssx[:, 2:3], ssx[:, 1:2], AF.Sqrt, [W + '1'], [W + '2'])
        P.op('dve', lambda e: e.reciprocal(out=ssx[:, 3:4], in_=ssx[:, 2:3]), [W + '2'], [W])

    def dbg_out(name, ap_sb, R):
        if name in dbg_t:
            ld('sp', dbg_t[name], ap_sb, R, ['dbg_' + name])

    SB = nc.alloc_sbuf_tensor
    idf = SB("idf", [128, 128], F32); idb = SB("idb", [128, 128], BF16)
    utf = SB("utf", [128, 128], F32); gtf = SB("gtf", [128, 128], F32)
    sutb = SB("sutb", [128, 128], BF16); onesf = SB("onesf", [128, 128], F32); onesb = SB("onesb", [128, 128], BF16)
    pswf = SB("pswf", [128, 128], F32); pswb = SB("pswb", [128, 128], BF16)
    signc = SB("signc", [128, 2], F32); mcol = SB("mcol", [128, 8], F32)
    tmpc = SB("tmpc", [128, 128], F32)
    dt_sb = SB("dt_sb", [128, NT, 32], F32)
    nwmix_s = SB("nwmix_s", [128, 16], F32); cw_s = SB("cw_s", [128, 32, 4], F32); cb_s = SB("cb_s", [128, 32], F32)
    dtb_s = SB("dtb_s", [128, 32], F32); aneg = SB("aneg", [128, 32], F32); dssd_s = SB("dssd_s", [128, 32], F32)
    nssd_s = SB("nssd_s", [128, 16], F32); gateb_s = SB("gateb_s", [128, 32], F32); d5_s = SB("d5_s", [128, 8], F32)
    Send5 = SB("Send5", [128, 64], F32)

    for (dst, src) in [(idf, c_id), (utf, c_ut), (gtf, c_gt), (pswf, c_psw), (signc, c_sign), (mcol, c_mcol),
                       (nwmix_s, nwmix), (cw_s, cw), (cb_s, cb), (dtb_s, dtb), (dssd_s, dssd), (nssd_s, nssd),
                       (gateb_s, gateb), (d5_s, d5), (tmpc, c_sut), (aneg, alog)]:
        ld('sp', dst[:], src, [], [dst.name])
    cp('dve', idb[:], idf[:], ['idf'], ['idb'])
    cp('dve', sutb[:], tmpc[:], ['tmpc'], ['sutb'])
    cp('dve', pswb[:], pswf[:], ['pswf'], ['pswb'])
    memset('dve', onesf[:], 1.0, ['onesf'])
    memset('dve', onesb[:], 1.0, ['onesb'])
    act(aneg[:], aneg[:], AF.Exp, ['aneg'], ['aneg'])
    ts('dve', aneg[:], aneg[:], -1.0, None, ALU.mult, None, ['aneg'], ['aneg'])
    P.barrier()

    def ssd_stage(full):
        with contextlib.ExitStack() as es:
            def SBs(name, shape, dt):
                return es.enter_context(nc.sbuf_tensor(name + ('_f' if full else '_p'), shape, dt))
            xs_t = [SBs(f"xs_t{i}", [128, 32, 64], BF16) for i in range(2)]
            btm_t = [SBs(f"btm_t{i}", [128, 1024], BF16) for i in range(2)]
            bfm_t = [SBs(f"bfm_t{i}", [128, 8, 128], BF16) for i in range(2)]
            cfm_t = [SBs(f"cfm_t{i}", [128, 8, 128], BF16) for i in range(2)]
            zs_t = [SBs(f"zs_t{i}", [128, 2048], BF16) for i in range(2)]
            S = SBs("S_ssd", [128, 32, 64], F32); Sh = SBs("Sh_ssd", [128, 32, 64], BF16)
            sm = {n: SBs("sm_" + n, [128, 32], F32) for n in ['raw', 'dtv', 'adt', 'acs', 'tot', 'eacs', 'dst', 'cd']}
            xdt = SBs("xdt", [128, 32, 64], BF16); xdts = SBs("xdts", [128, 32, 64], BF16)
            Gm = SBs("Gm", [128, 128], F32)
            lhTa = SBs("lhTa", [128, 32, 128], F32)
            E4 = [SBs(f"E4_{i}", [128, 512], F32) for i in range(2)]
            Gh4 = [SBs(f"Gh4_{i}", [128, 4, 128], BF16) for i in range(2)]
            ytile = SBs("ytile", [128, 32, 64], F32); ytmp = SBs("ytmp", [128, 32, 64], F32)
            yn = SBs("yn", [128, 2048], BF16)
            ynTt = [SBs(f"ynTt{i}", [128, 16, 128], BF16) for i in range(2)]
            ssS = SBs("ssS", [128, 4], F32)

            Sall = ['S'] + [f'S{g}' for g in range(8)]
            totsum = SBs("totsum", [128, 32], F32)
            xg = [SBs(f"xg{i}", [128, XW], F32) for i in range(2)]
            mk_s = SBs("mk_s", [128, 8], F32)
            memset('dve', S[:], 0.0, Sall)
            memset('dve', totsum[:], 0.0, ['totsum'])
            if full:
                ld('sp', mk_s[:], c_mk, [], ['mk_s'])
                S2 = S[:].rearrange("p h q -> p (h q)")
                for m_ in range(ncores - 1):
                    jx = m_ % 2
                    ld('sp', xg[jx][:], xch_dst[m_ * 128:(m_ + 1) * 128, :], [], [f'xg{jx}'])
                    tt('dve', ytmp[:], S[:], xg[jx][:, 2048:2080].unsqueeze(2).to_broadcast([128, 32, 64]), ALU.mult, Sall + [f'xg{jx}'], ['ytmp'])
                    yt_ = ytmp[:].rearrange("p h q -> p (h q)")
                    tt('dve', yt_, yt_, xg[jx][:, 0:2048], ALU.add, ['ytmp', f'xg{jx}'], ['ytmp'])
                    tt('dve', yt_, yt_, S2, ALU.subtract, ['ytmp'] + Sall, ['ytmp'])
                    stt(S2, yt_, mk_s[:, m_:m_ + 1], S2, ALU.mult, ALU.add, ['ytmp', 'mk_s'] + Sall, Sall)
            cp('act', Sh[:], S[:], Sall, [f'Sh{g}' for g in range(8)])
            hc = 0
            wac = 0
            woc = 0
            for ti in range(NT):
                b = ti % 2
                r0 = ti * 128
                ld('sp', xs_t[b][:].rearrange("p h q -> p (h q)"), xs_d[r0:r0 + 128, :], [], [f'xs_t{b}'])
                ld('sp', btm_t[b][:], btm_d[r0:r0 + 128, :], [], [f'btm_t{b}'])
                if full:
                  ld('sp', bfm_t[b][:], xc_d[2048:3072, r0:r0 + 128].rearrange("(g n) t -> n g t", n=128), [], [f'bfm_t{b}'])
                if full:
                  ld('sp', cfm_t[b][:], xc_d[3072:4096, r0:r0 + 128].rearrange("(g n) t -> n g t", n=128), [], [f'cfm_t{b}'])
                if full:
                  ld('sp', zs_t[b][:], zs_d[r0:r0 + 128, :], [], [f'zs_t{b}'])
                tt('dve', sm['raw'][:], dt_sb[:, ti, :], dtb_s[:], ALU.add, ['dtb_s'], ['sm_raw'])
                act(sm['raw'][:], sm['raw'][:], AF.Exp, ['sm_raw'], ['sm_raw'])
                ts('dve', sm['raw'][:], sm['raw'][:], 1.0, None, ALU.add, None, ['sm_raw'], ['sm_raw'])
                act(sm['dtv'][:], sm['raw'][:], AF.Ln, ['sm_raw'], ['sm_dtv'])
                tt('dve', sm['adt'][:], sm['dtv'][:], aneg[:], ALU.mult, ['sm_dtv'], ['sm_adt'])
                mm(bank(0)[:, 0:32], utf[:], sm['adt'][:], True, True, ['utf', 'sm_adt'], ['bank0'])
                mm(bank(0)[:, 32:64], onesf[:], sm['adt'][:], True, True, ['onesf', 'sm_adt'], ['bank0'])
                cp('dve', sm['acs'][:], bank(0)[:, 0:32], ['bank0'], ['sm_acs'])
                cp('dve', sm['tot'][:], bank(0)[:, 32:64], ['bank0'], ['sm_tot'])
                tt('dve', totsum[:], totsum[:], sm['tot'][:], ALU.add, ['totsum', 'sm_tot'], ['totsum'])
                act(sm['eacs'][:], sm['acs'][:], AF.Exp, ['sm_acs'], ['sm_eacs'])
                tt('dve', sm['dst'][:], sm['tot'][:], sm['acs'][:], ALU.subtract, ['sm_tot', 'sm_acs'], ['sm_dst'])
                act(sm['dst'][:], sm['dst'][:], AF.Exp, ['sm_dst'], ['sm_dst'])
                act(sm['cd'][:], sm['tot'][:], AF.Exp, ['sm_tot'], ['sm_cd'])
                tt('dve', xdt[:], xs_t[b][:], sm['dtv'][:].unsqueeze(2).to_broadcast([128, 32, 64]), ALU.mult, [f'xs_t{b}', 'sm_dtv'], ['xdt'])
                tt('dve', xdts[:], xdt[:], sm['dst'][:].unsqueeze(2).to_broadcast([128, 32, 64]), ALU.mult, ['xdt', 'sm_dst'], ['xdts'])
                if full:
                    tt('dve', lhTa[:], gtf[:].unsqueeze(1).to_broadcast([128, 32, 128]),
                       sm['adt'][:].unsqueeze(2).to_broadcast([128, 32, 128]), ALU.mult, ['gtf', 'sm_adt'], ['lhTa'])
                for g in range(8):
                    p_ = g % 2
                    if full:
                        mm(bank(1)[:, 0:128], bfm_t[b][:, g, :], cfm_t[b][:, g, :], True, True, [f'bfm_t{b}', f'cfm_t{b}'], ['bank1'])
                        tt('dve', Gm[:], bank(1)[:, 0:128], utf[:], ALU.mult, ['bank1', 'utf'], ['Gm'])
                        for r in range(4):
                            mm(bank(2 + p_)[:, r * 128:(r + 1) * 128], lhTa[:, 4 * g + r, :], utf[:], True, True, ['lhTa', 'utf'], [f'bank{2 + p_}'])
                        act(E4[p_][:], bank(2 + p_), AF.Exp, [f'bank{2 + p_}'], [f'E4{p_}'])
                        tt('dve', Gh4[p_][:], E4[p_][:].rearrange("p (a q) -> p a q", a=4), Gm[:].unsqueeze(1).to_broadcast([128, 4, 128]),
                           ALU.mult, [f'E4{p_}', 'Gm'], [f'Gh4{p_}'])
                    for r in range(4):
                        h = 4 * g + r
                        mm(bank(6)[:, r * 64:(r + 1) * 64], btm_t[b][:, g * 128:(g + 1) * 128], xdts[:, h, :], True, True, [f'btm_t{b}', 'xdts'], ['bank6'])
                        if not full:
                            continue
                        mm(bank(4 + p_)[:, r * 64:(r + 1) * 64], Gh4[p_][:, r, :], xdt[:, h, :], True, True, [f'Gh4{p_}', 'xdt'], [f'bank{4 + p_}'])
                        mm(bank(4 + p_)[:, 256 + r * 64:256 + (r + 1) * 64], cfm_t[b][:, g, :], Sh[:, h, :], True, True, [f'cfm_t{b}', f'Sh{g}'], [f'bank{4 + p_}'])
                    yv = ytile[:, 4 * g:4 * g + 4, :]
                    if full:
                        tt('dve', yv, bank(4 + p_)[:, 256:512].rearrange("p (a q) -> p a q", a=4),
                           sm['eacs'][:, 4 * g:4 * g + 4].unsqueeze(2).to_broadcast([128, 4, 64]), ALU.mult, [f'bank{4 + p_}', 'sm_eacs'], [f'yt{g}'])
                        tt('dve', yv, yv, bank(4 + p_)[:, 0:256].rearrange("p (a q) -> p a q", a=4), ALU.add, [f'bank{4 + p_}', f'yt{g}'], [f'yt{g}'])
                    Sv = S[:, 4 * g:4 * g + 4, :]
                    tt('dve', Sv, Sv, sm['cd'][:, 4 * g:4 * g + 4].unsqueeze(2).to_broadcast([128, 4, 64]), ALU.mult, [f'S{g}', 'sm_cd'], [f'S{g}'])
                    tt('dve', Sv, Sv, bank(6)[:, 0:256].rearrange("p (a q) -> p a q", a=4), ALU.add, [f'S{g}', 'bank6'], [f'S{g}'])
                    cp('act', Sh[:, 4 * g:4 * g + 4, :], Sv, [f'S{g}'], [f'Sh{g}'])
                if not full:
                    continue
                yts = [f'yt{g}' for g in range(8)]
                tt('dve', ytmp[:], xs_t[b][:], dssd_s[:].unsqueeze(2).to_broadcast([128, 32, 64]), ALU.mult, [f'xs_t{b}', 'dssd_s'], ['ytmp'])
                tt('dve', ytile[:], ytile[:], ytmp[:], ALU.add, yts + ['ytmp'], ['ytile'])
                if 'yssd' in dbg_t:
                    ld('sp', dbg_t['yssd'][r0:r0 + 128, :], ytile[:].rearrange("p h q -> p (h q)"), ['ytile'], ['dbgy'])
                yt2 = ytile[:].rearrange("p h q -> p (h q)")
                tt('dve', yt2, yt2, zs_t[b][:], ALU.mult, ['ytile', f'zs_t{b}'], ['ytile'])
                act(ytmp[:].rearrange("p h q -> p (h q)"), yt2, AF.Square, ['ytile'], ['ytmp', 'ssS0'], accum=ssS[:, 0:1])
                rstd_from_ss(ssS, ['ssS0'], 'ssSr')
                act(yn[:], yt2, AF.Copy, ['ytile', 'ssSr'], ['yn'], scale=ssS[:, 3:4])
                yb2 = ti % 2
                for half in range(2):
                    pb = bank(7).bitcast(BF16)
                    for k in range(8):
                        kt = half * 8 + k
                        tr(pb[:, k * 128:(k + 1) * 128], yn[:, kt * 128:(kt + 1) * 128], idb[:], ['yn', 'idb'], ['bank7'])
                    for k in range(8):
                        kt = half * 8 + k
                        ts('dve', ynTt[yb2][:, kt, :], pb[:, k * 128:(k + 1) * 128], nssd_s[:, kt:kt + 1], None,
                           ALU.mult, None, ['bank7', 'nssd_s'], [f'ynTt{yb2}'])
                ld('sp', yn_d.rearrange("(k p) t -> p k t", p=128)[:, :, r0:r0 + 128], ynTt[yb2][:], [f'ynTt{yb2}'], ['yn_d'])
            if not full:
                ld('sp', xch_src[:, 0:2048], S[:].rearrange("p h q -> p (h q)"), ['S'] + [f'S{g}' for g in range(8)], ['xch_src_a'])
                act(totsum[:], totsum[:], AF.Exp, ['totsum'], ['totsum'])
                ld('sp', xch_src[:, 2048:2080], totsum[:], ['totsum'], ['xch_src_b'])
            P.barrier()


    def proj_stage():
        with contextlib.ExitStack() as es:
            def SBs(name, shape, dt):
                return es.enter_context(nc.sbuf_tensor(name, shape, dt))
            mT = SBs("mT", [128, 16, TOK], BF16)
            with contextlib.ExitStack() as es2:
                def SB2(name, shape, dt):
                    return es2.enter_context(nc.sbuf_tensor(name, shape, dt))
                ynT = SB2("ynTg", [128, 16, TOK], BF16)
                wa_t = [SB2(f"wa_t{i}", [128, 16, 128], BF16) for i in range(2)]
                gat = [SB2(f"gat{i}", [128, TOK], BF16) for i in range(2)]
                ybt = [SB2(f"ybt{i}", [128, TOK], BF16) for i in range(2)]
                mtmp = [SB2(f"mtmp{i}", [128, 512], F32) for i in range(2)]
                for hq in range(4):
                    ld('sp', ynT[:, :, hq * 512:(hq + 1) * 512], yn_d.rearrange("(k p) t -> p k t", p=128)[:, :, hq * 512:(hq + 1) * 512], [], [f'ynTg{hq}'])
                c_ = 0
                for dtile in range(16):
                    j = dtile % 2
                    ld('pool', wa_t[j][:], w_a[:, :, dtile * 128:(dtile + 1) * 128], [], [f'wa_t{j}'])
                    ld('sp', gat[j][:], gs_d[dtile * 128:(dtile + 1) * 128, :], [], [f'gat{j}'])
                    ld('sp', ybt[j][:], ybg_d[dtile * 128:(dtile + 1) * 128, :], [], [f'ybt{j}'])
                    for tg in range(4):
                        bk = c_ % 4
                        mj = c_ % 2
                        c_ += 1
                        sl = slice(tg * 512, (tg + 1) * 512)
                        for kt in range(16):
                            mm(bank(bk), wa_t[j][:, kt, :], ynT[:, kt, sl], kt == 0, kt == 15, [f'wa_t{j}', f'ynTg{tg}'], [f'bank{bk}'])
                        tt('dve', mtmp[mj][:], bank(bk), gat[j][:, sl], ALU.mult, [f'bank{bk}', f'gat{j}'], [f'mtmp{mj}'])
                        tt('dve', mT[:, dtile, sl], mtmp[mj][:], ybt[j][:, sl], ALU.add, [f'mtmp{mj}', f'ybt{j}'], [f'mT{tg}'])
                P.barrier()
            wo_t = [SBs(f"wo_t{i}", [128, 16, 512], BF16) for i in range(2)]
            xres = [SBs(f"xres{i}", [128, 512], F32) for i in range(4)]
            x1t = [SBs(f"x1t{i}", [128, 512], F32) for i in range(4)]
            xc_ = 0
            for cg in range(4):
                j = cg % 2
                ld('pool', wo_t[j][:], w_o[:, :, cg * 512:(cg + 1) * 512], [], [f'wo_t{j}'])
                for ti in range(NT):
                    jj = xc_ % 4
                    xc_ += 1
                    rr = ti * 128
                    ld('sp', xres[jj][:], x_c[rr:rr + 128, cg * 512:(cg + 1) * 512], [], [f'xres{jj}'])
                    for kt in range(16):
                        mm(bank(4 + jj), mT[:, kt, rr:rr + 128], wo_t[j][:, kt, :], kt == 0, kt == 15, [f'wo_t{j}'], [f'bank{4 + jj}'])
                    tt('dve', x1t[jj][:], bank(4 + jj), xres[jj][:], ALU.add, [f'bank{4 + jj}', f'xres{jj}'], [f'x1t{jj}'])
                    ld('sp', x1_d[rr:rr + 128, cg * 512:(cg + 1) * 512], x1t[jj][:], [f'x1t{jj}'], ['x1_d'])
            P.barrier()

    with contextlib.ExitStack() as es:
        def SBs(name, shape, dt):
            return es.enter_context(nc.sbuf_tensor(name, shape, dt))
        hT = SBs("hT", [128, 16, 128 + TOK], BF16)
        xt = [SBs(f"xt{i}", [128, D], F32) for i in range(2)]
        xn = [SBs(f"xn{i}", [128, D], BF16) for i in range(2)]
        junk = SBs("junkA", [128, D], BF16)
        ssA = [SBs(f"ssA{i}", [128, 4], F32) for i in range(2)]
        wbuf = [SBs(f"wbuf{i}", [128, 16, 512], BF16) for i in range(2)]
        wdt_s = SBs("wdt_s", [128, 16, 32], BF16)
        pc = [SBs(f"pc{i}", [128, 8 + TOK], F32) for i in range(2)]
        cacc = [SBs(f"cacc{i}", [128, TOK], F32) for i in range(2)]
        cv = [SBs(f"cv{i}", [128, TOK], BF16) for i in range(2)]
        stg = [SBs(f"stg{i}", [128, NT, 128], BF16) for i in range(2)]
        zo = [SBs(f"zo{i}", [128, 512], BF16) for i in range(2)]

        for ti in range(NT + 1):
            b = ti % 2
            src = x_h if ti == 0 else x_c[(ti - 1) * 128: ti * 128, :]
            ld('sp', xt[b][:], src, [], [f'xt{b}'])
            act(junk[:], xt[b][:], AF.Square, [f'xt{b}'], ['junkA', f'ssA{b}0'], accum=ssA[b][:, 0:1])
            rstd_from_ss(ssA[b], [f'ssA{b}0'], f'ssA{b}r')
            act(xn[b][:], xt[b][:], AF.Copy, [f'xt{b}', f'ssA{b}r'], [f'xn{b}'], scale=ssA[b][:, 3:4])
            for half in range(2):
                pb = bank(half).bitcast(BF16)
                for k in range(8):
                    kt = half * 8 + k
                    tr(pb[:, k * 128:(k + 1) * 128], xn[b][:, kt * 128:(kt + 1) * 128], idb[:], [f'xn{b}', 'idb'], [f'bank{half}'])
                for k in range(8):
                    kt = half * 8 + k
                    ts('dve', hT[:, kt, ti * 128:(ti + 1) * 128], pb[:, k * 128:(k + 1) * 128], nwmix_s[:, kt:kt + 1], None,
                       ALU.mult, None, [f'bank{half}'], [f'hT{ti}'])
        hT_all = [f'hT{ti}' for ti in range(NT + 1)]
        wi = [0]

        def load_w(src_ap):
            b = wi[0] % 2
            wi[0] += 1
            ld('pool', wbuf[b][:], src_ap, [], [f'wbuf{b}'])
            return wbuf[b], f'wbuf{b}'

        zc = 0
        for cg in range(4):
            wt_, wn = load_w(w_z[:, :, cg * 512:(cg + 1) * 512])
            for ti in range(NT):
                bk = 2 + (zc % 2)
                for kt in range(16):
                    mm(bank(bk), hT[:, kt, (ti + 1) * 128:(ti + 2) * 128], wt_[:, kt, :], kt == 0, kt == 15,
                       [f'hT{ti + 1}', wn], [f'bank{bk}'])
                zb = zc % 2
                act(zo[zb][:], bank(bk), AF.Silu, [f'bank{bk}'], [f'zo{zb}'])
                ld('sp', zs_d[ti * 128:(ti + 1) * 128, cg * 512:(cg + 1) * 512], zo[zb][:], [f'zo{zb}'], ['zs_d'])
                zc += 1
        ld('pool', wdt_s[:], w_dt, [], ['wdt_s'])
        for ti in range(NT):
            for kt in range(16):
                mm(bank(4)[:, 0:32], hT[:, kt, (ti + 1) * 128:(ti + 2) * 128], wdt_s[:, kt, :], kt == 0, kt == 15,
                   [f'hT{ti + 1}', 'wdt_s'], ['bank4'])
            cp('dve', dt_sb[:, ti, :], bank(4)[:, 0:32], ['bank4'], ['dt_sb'])

        def fm_group(wsrc, ncol_tiles, kind):
            cnt = 0
            for c4 in range(ncol_tiles // 4):
                wt_, wn = load_w(wsrc[:, :, c4 * 512:(c4 + 1) * 512])
                for cc in range(4):
                    ct = c4 * 4 + cc
                    pb_ = cnt % 2
                    cnt += 1
                    if kind == 'xbc':
                        for kt in range(16):
                            mm(bank(6)[:, 0:8], wt_[:, kt, cc * 128:(cc + 1) * 128], hT[:, kt, 120:128], kt == 0, kt == 15,
                               ['hT0', wn], ['bank6'])
                        cp('act', pc[pb_][:, 0:8], bank(6)[:, 0:8], ['bank6'], [f'pc{pb_}h'])
                    for tg in range(4):
                        bk = 2 + ((cnt * 4 + tg) % 4)
                        for kt in range(16):
                            mm(bank(bk), wt_[:, kt, cc * 128:(cc + 1) * 128], hT[:, kt, 128 + tg * 512: 128 + (tg + 1) * 512],
                               kt == 0, kt == 15, hT_all + [wn], [f'bank{bk}'])
                        if kind == 'xbc':
                            cp('act', pc[pb_][:, 8 + tg * 512: 8 + (tg + 1) * 512], bank(bk), [f'bank{bk}'], [f'pc{pb_}{tg}'])
                        elif kind == 'u':
                            cp('act', cv[pb_][:, tg * 512:(tg + 1) * 512], bank(bk), [f'bank{bk}'], [f'cv{pb_}'])
                        else:
                            act(cv[pb_][:, tg * 512:(tg + 1) * 512], bank(bk), AF.Sigmoid, [f'bank{bk}', 'gateb_s'], [f'cv{pb_}'],
                                bias=gateb_s[:, ct:ct + 1])
                    if kind == 'xbc':
                        pcr = [f'pc{pb_}h'] + [f'pc{pb_}{tg}' for tg in range(4)]
                        a_ = cacc[pb_]
                        ts('dve', a_[:], pc[pb_][:, 8:8 + TOK], cw_s[:, ct, 3:4], cb_s[:, ct:ct + 1], ALU.mult, ALU.add, pcr, [f'cacc{pb_}'])
                        for k in range(3):
                            stt(a_[:], pc[pb_][:, 5 + k:5 + k + TOK], cw_s[:, ct, k:k + 1], a_[:], ALU.mult, ALU.add,
                                pcr + [f'cacc{pb_}'], [f'cacc{pb_}'])
                        act(cv[pb_][:], a_[:], AF.Silu, [f'cacc{pb_}'], [f'cv{pb_}'])
                        ld('sp', xc_d[ct * 128:(ct + 1) * 128, :], cv[pb_][:], [f'cv{pb_}'], ['xc_d'])
                        if ct < 24:
                            for q4 in range(4):
                                bk2 = q4 % 2
                                pbb = bank(bk2).bitcast(BF16)
                                for j in range(4):
                                    ti = q4 * 4 + j
                                    tr(pbb[:, j * 128:(j + 1) * 128], cv[pb_][:, ti * 128:(ti + 1) * 128], idb[:], [f'cv{pb_}', 'idb'], [f'bank{bk2}'])
                                cp('dve', stg[pb_][:, q4 * 4:(q4 + 1) * 4, :], pbb[:, 0:512].rearrange("p (a b) -> p a b", a=4),
                                   [f'bank{bk2}'], [f'stg{pb_}'])
                            if ct < 16:
                                dst = xs_d[:, ct * 128:(ct + 1) * 128].rearrange("(t p) c -> p t c", p=128)
                                dn = 'xs_d'
                            else:
                                dst = btm_d[:, (ct - 16) * 128:(ct - 15) * 128].rearrange("(t p) c -> p t c", p=128)
                                dn = 'btm_d'
                            ld('sp', dst, stg[pb_][:], [f'stg{pb_}'], [dn])
                    elif kind == 'u':
                        ld('sp', u_d[ct * 128:(ct + 1) * 128, :], cv[pb_][:], [f'cv{pb_}'], ['u_d'])
                    else:
                        ld('sp', gs_d[ct * 128:(ct + 1) * 128, :], cv[pb_][:], [f'cv{pb_}'], ['gs_d'])
        fm_group(w_xbc, 32, 'xbc')
        fm_group(w_u, 8, 'u')
        fm_group(w_g, 32, 'g')
        P.barrier()
    if stop_after == 'AB':
        if 'dt' in dbg_t:
            ld('sp', dbg_t['dt'], dt_sb[:, 0, :], [], ['o1'])
        P.finish()
        return nc


    ssd_stage(False)
    if stop_after == 'SSD1':
        P.finish()
        return nc

    PW_N = 12
    with contextlib.ExitStack() as es:
        def SBs(name, shape, dt):
            return es.enter_context(nc.sbuf_tensor(name, shape, dt))
        lr_s = SBs("lr_s", [128, 64], F32); li_s = SBs("li_s", [128, 64], F32); dt5 = SBs("dt5", [128, 64], F32)
        t5 = [SBs(f"t5_{i}", [128, 64], F32) for i in range(8)]
        pw_r = SBs("pw_r", [128, PW_N, 64], F32); pw_i = SBs("pw_i", [128, PW_N, 64], F32); aix = SBs("aix", [128, PW_N, 64], F32)
        b1_s = SBs("b1_s", [128, 1024], F32); b2_s = SBs("b2_s", [128, 1024], F32); BB = SBs("BB", [128, 1024], F32)
        c1_s = SBs("c1_s", [128, 1024], F32)
        BBTp = SBs("BBTp", [128, 64, 128], BF16); CTp = SBs("CTp", [128, 64, 128], BF16)
        u_fm = SBs("u_fm", [128, 8, TOK], BF16)
        Xh = SBs("Xh5", [128, TOK], BF16)
        lkt = SBs("lkt", [128, 128], F32); lk = [SBs(f"lk{i}", [128, 128], BF16) for i in range(4)]
        y5t = [SBs(f"y5t{i}", [128, TOK], F32) for i in range(2)]

        ld('sp', lr_s[:], lre, [], ['lr_s']); ld('sp', li_s[:], lim, [], ['li_s']); ld('sp', dt5[:], ldt, [], ['dt5'])
        ld('sp', b1_s[:], b1.rearrange("p g c -> p (g c)"), [], ['b1_s']); ld('sp', b2_s[:], b2.rearrange("p g c -> p (g c)"), [], ['b2_s'])
        ld('sp', c1_s[:], c1.rearrange("p g c -> p (g c)"), [], ['c1_s'])
        ld('sp', u_fm[:], u_d.rearrange("(a p) t -> p a t", p=128), [], ['u_fm'])
        act(dt5[:], dt5[:], AF.Exp, ['dt5'], ['dt5'])
        mag, ang, sn, cs, ta, tb, tc, td = [t[:] for t in t5]
        tt('dve', mag, lr_s[:], dt5[:], ALU.mult, ['lr_s', 'dt5'], ['mag'])
        act(mag, mag, AF.Exp, ['mag'], ['mag'])
        tt('dve', ang, li_s[:], dt5[:], ALU.mult, ['li_s', 'dt5'], ['ang'])
        act(sn, ang, AF.Sin, ['ang'], ['sn'], scale=0.125)
        ts('dve', ta, ang, -0.125, math.pi / 2, ALU.mult, ALU.add, ['ang'], ['ta'])
        act(cs, ta, AF.Sin, ['ta'], ['cs'])
        for _ in range(3):
            tt('dve', ta, cs, cs, ALU.mult, ['cs'], ['ta'])
            tt('dve', tb, sn, sn, ALU.mult, ['sn'], ['tb'])
            tt('dve', tc, cs, sn, ALU.mult, ['cs', 'sn'], ['tc'])
            tt('dve', cs, ta, tb, ALU.subtract, ['ta', 'tb'], ['cs'])
            ts('dve', sn, tc, 2.0, None, ALU.mult, None, ['tc'], ['sn'])
        tt('dve', pw_r[:, 0, :], mag, cs, ALU.mult, ['mag', 'cs'], ['pw'])
        tt('dve', pw_i[:, 0, :], mag, sn, ALU.mult, ['mag', 'sn'], ['pw'])
        for k in range(1, PW_N):
            tt('dve', ta, pw_r[:, k - 1, :], pw_r[:, k - 1, :], ALU.mult, ['pw'], ['ta'])
            tt('dve', tb, pw_i[:, k - 1, :], pw_i[:, k - 1, :], ALU.mult, ['pw'], ['tb'])
            tt('dve', tc, pw_r[:, k - 1, :], pw_i[:, k - 1, :], ALU.mult, ['pw'], ['tc'])
            tt('dve', pw_r[:, k, :], ta, tb, ALU.subtract, ['ta', 'tb'], ['pw'])
            ts('dve', pw_i[:, k, :], tc, 2.0, None, ALU.mult, None, ['tc'], ['pw'])
        ts('dve', aix[:].rearrange("p k g -> p (k g)"), pw_i[:].rearrange("p k g -> p (k g)"), signc[:, 0:1], None, ALU.mult, None, ['pw', 'signc'], ['aix'])
        nr = td
        ts('dve', nr, pw_r[:, 0, :], -1.0, None, ALU.add, None, ['pw'], ['nr'])
        tt('dve', ta, lr_s[:], lr_s[:], ALU.mult, ['lr_s'], ['ta'])
        tt('dve', tb, li_s[:], li_s[:], ALU.mult, ['li_s'], ['tb'])
        tt('dve', ta, ta, tb, ALU.add, ['ta', 'tb'], ['ta'])
        P.op('dve', lambda e: e.reciprocal(out=tc, in_=ta), ['ta'], ['tc'])
        tt('dve', ta, nr, lr_s[:], ALU.mult, ['nr', 'lr_s'], ['ta'])
        tt('dve', tb, pw_i[:, 0, :], li_s[:], ALU.mult, ['pw', 'li_s'], ['tb'])
        tt('dve', ta, ta, tb, ALU.add, ['ta', 'tb'], ['ta'])
        tt('dve', mag, ta, tc, ALU.mult, ['ta', 'tc'], ['mag'])
        tt('dve', ta, pw_i[:, 0, :], lr_s[:], ALU.mult, ['pw', 'lr_s'], ['ta'])
        tt('dve', tb, nr, li_s[:], ALU.mult, ['nr', 'li_s'], ['tb'])
        tt('dve', ta, ta, tb, ALU.subtract, ['ta', 'tb'], ['ta'])
        tt('dve', ang, ta, tc, ALU.mult, ['ta', 'tc'], ['ang'])
        ts('dve', ang, ang, signc[:, 1:2], None, ALU.mult, None, ['ang', 'signc'], ['ang'])
        b13 = b1_s[:].rearrange("p (g c) -> p g c", c=16); b23 = b2_s[:].rearrange("p (g c) -> p g c", c=16)
        BB3 = BB[:].rearrange("p (g c) -> p g c", c=16)
        tt('dve', b13, b13, mag.unsqueeze(2).to_broadcast([128, 64, 16]), ALU.mult, ['b1_s', 'mag'], ['b1_s'])
        tt('dve', b23, b23, ang.unsqueeze(2).to_broadcast([128, 64, 16]), ALU.mult, ['b2_s', 'ang'], ['b2_s'])
        tt('dve', BB[:], b1_s[:], b2_s[:], ALU.add, ['b1_s', 'b2_s'], ['BB'])
        for a in range(8):
            tr(bank(0)[:, 0:128], BB[:, a * 128:(a + 1) * 128], idf[:], ['BB', 'idf'], ['bank0'])
            for g8 in range(8):
                ts('dve', BBTp[:, a * 8 + g8, :], bank(0)[:, 0:128], mcol[:, g8:g8 + 1], None, ALU.mult, None, ['bank0', 'mcol'], ['BBTp'])
        memset('pool', CTp[:], 0.0, ['CTp'])
        c13 = c1_s[:].rearrange("p (g c) -> p g c", c=16)
        for b in range(8):
            ts('dve', CTp[:, b::8, 16 * b:16 * b + 16], c13[:, b::8, :], signc[:, 0:1], None, ALU.mult, None, ['c1_s', 'signc', 'CTp'], ['CTp'])

        P.barrier()

        def make_lk(g, k, j):
            ts('dve', lkt[:], idf[:], pw_r[:, k, g:g + 1], None, ALU.mult, None, ['idf', 'pw'], ['lkt'])
            stt(lk[j][:], pswf[:], aix[:, k, g:g + 1], lkt[:], ALU.mult, ALU.add, ['pswf', 'aix', 'lkt'], [f'lk{j}'])

        lkc = 0
        Xh4 = [[Xh, SBs("Xh5b", [128, TOK], BF16)], [SBs("Xh5c", [128, TOK], BF16), SBs("Xh5d", [128, TOK], BF16)]]
        conv_list = []
        for ex_ in range(32):
            conv_list.append((weg_d[ex_], w_eg[ex_].rearrange("p k n -> (p k) n")))
            conv_list.append((weu_d[ex_], w_eu[ex_].rearrange("p k n -> (p k) n")))
            conv_list.append((wed_d[ex_], w_ed[ex_].rearrange("p k n -> (p k) n")))
        conv_i = [0]

        def issue_conv(n):
            for _ in range(n):
                if conv_i[0] < len(conv_list):
                    dst_, src_ = conv_list[conv_i[0]]
                    P.dma('pool', lambda e, dst_=dst_, src_=src_: e.dma_start(out=dst_, in_=src_, max_dma_last_dim=2048), [], [f'wconv{conv_i[0]}'], bg=True)
                    conv_i[0] += 1
        XhF = SBs("XhF", [128, 8, TOK], BF16)
        for g in range(64):
            issue_conv(2 if g % 2 == 0 else 1)
            a, g8 = g // 8, g % 8
            PX = PA if g % 2 == 0 else PB
            pn = 'PAb' if g % 2 == 0 else 'PBb'
            Xh2 = Xh4[g % 2]
            xq = g % 2
            hcur = 0
            for nb in range(4):
                sl = slice(nb * 512, (nb + 1) * 512)
                mm(PX[:, sl], BBTp[:, g, :], u_fm[:, a, sl], True, False, ['BBTp', 'u_fm'], [f'{pn}{nb}'])
                cp('act', Xh2[0][:, sl], PX[:, sl], [f'{pn}{nb}'], [f'Xh{xq}_0c{nb}'])
            for k in range(11):
                d = 1 << k
                j = lkc % 4
                lkc += 1
                make_lk(g, k, j)
                hn = 1 - hcur
                last = (k == 10)
                for bk in range(4):
                    lo, hi = max(d, 512 * bk), 512 * (bk + 1)
                    sl = slice(bk * 512, (bk + 1) * 512)
                    if lo < hi:
                        rd = sorted({(lo - d) // 512, (hi - d - 1) // 512})
                        mm(PX[:, lo:hi], lk[j][:], Xh2[hcur][:, lo - d:hi - d], False, last,
                           [f'lk{j}'] + [f'Xh{xq}_{hcur}c{c}' for c in rd], [f'{pn}{bk}'])
                    if last:
                        cp('act', XhF[:, g8, sl], PX[:, sl], [f'{pn}{bk}'], [f'XhF{g8}c{bk}'])
                    else:
                        cp('act' if bk % 2 == 0 else 'dve', Xh2[hn][:, sl], PX[:, sl], [f'{pn}{bk}'], [f'Xh{xq}_{hn}c{bk}'])
                hcur = hn
            cp('dve', Send5[:, g:g + 1], PX[:, TOK - 1:TOK], [f'{pn}3'], ['Send5'])
            if g8 == 7:
                for nb in range(4):
                    sl = slice(nb * 512, (nb + 1) * 512)
                    for q in range(8):
                        mm(PA[:, sl], CTp[:, a * 8 + q, :], XhF[:, q, sl], q == 0, q == 7, ['CTp', f'XhF{q}c{nb}'], [f'PAb{nb}'])
                yb_ = a % 2
                stt(y5t[yb_][:], u_fm[:, a, :], d5_s[:, a:a + 1], PA[:, :], ALU.mult, ALU.add,
                    ['u_fm', 'd5_s'] + [f'PAb{nb}' for nb in range(4)], [f'y5t{yb_}'])
                ld('sp', y5_d[a * 128:(a + 1) * 128, :], y5t[yb_][:], [f'y5t{yb_}'], ['y5_d'])
        ld('sp', xch_src[:, 2080:2144], Send5[:], ['Send5'], ['xch_src_c'])
        P.barrier()
        if ncores > 1:
            P.cc(lambda e: e.collective_compute("AllGather", ALU.bypass, replica_groups=[list(range(ncores))],
                                                ins=[xch_src.opt()], outs=[xch_dst[0:ncores * 128, :].opt()]), [], ['xch_dst'])
        P.barrier()
        if ncores > 1:
            Si = SBs("Si5", [128, 64], F32); Sih = SBs("Sih5", [128, 66], BF16)
            swl = SBs("swl", [128, 128], F32)
            ge5 = [SBs(f"ge5_{i}", [128, 64], F32) for i in range(2)]
            mk5 = SBs("mk5", [128, 8], F32)
            ld('sp', mk5[:], c_mk, [], ['mk5'])
            ts('dve', swl[:], pswf[:], signc[:, 1:2], None, ALU.mult, None, ['pswf', 'signc'], ['swl'])
            memset('dve', Si[:], 0.0, ['Si'])
            memset('dve', Sih[:], 0.0, ['Sih'])
            for m_ in range(ncores - 1):
                jx = m_ % 2
                ld('sp', ge5[jx][:], xch_dst[m_ * 128:(m_ + 1) * 128, 2080:2144], [], [f'ge5{jx}'])
                mm(bank(0)[:, 0:64], swl[:], Si[:], True, True, ['swl', 'Si'], ['bank0'])
                tt('dve', ta, pw_r[:, 11, :], Si[:], ALU.mult, ['pw', 'Si'], ['ta'])
                tt('dve', tb, bank(0)[:, 0:64], pw_i[:, 11, :], ALU.mult, ['pw', 'bank0'], ['tb'])
                tt('dve', ta, ta, tb, ALU.subtract, ['ta', 'tb'], ['ta'])
                tt('dve', ta, ta, ge5[jx][:], ALU.add, ['ta', f'ge5{jx}'], ['ta'])
                tt('dve', ta, ta, Si[:], ALU.subtract, ['ta', 'Si'], ['ta'])
                stt(Si[:], ta, mk5[:, m_:m_ + 1], Si[:], ALU.mult, ALU.add, ['ta', 'mk5', 'Si'], ['Si'])
            cp('dve', Sih[:, 0:64], Si[:], ['Si'], ['Sih'])
            rot = 0
            ce = 0
            for a in range(8):
                for q in range(8):
                    g = 8 * a + q
                    bk = rot % 4
                    rot += 1
                    for k in range(2):
                        j = lkc % 4
                        lkc += 1
                        make_lk(g, k, j)
                        mm(PB[:, bk * 512 + 2 * k:bk * 512 + 2 * k + 2], lk[j][:], Sih[:, g:g + 2], True, True, [f'lk{j}', 'Sih'], [f'PBb{bk}'])
                    cp('dve', XhF[:, q, 0:2], PB[:, bk * 512:bk * 512 + 4:2], [f'PBb{bk}'], [f'Z{q}'])
                for k in range(1, 11):
                    d = 1 << k
                    for q in range(8):
                        g = 8 * a + q
                        j = lkc % 4
                        lkc += 1
                        make_lk(g, k, j)
                        for hh in range(max(1, d // 512)):
                            w_ = min(d, 512)
                            bk = rot % 4
                            rot += 1
                            ce += 1
                            mm(PB[:, bk * 512:bk * 512 + w_], lk[j][:], XhF[:, q, hh * 512:hh * 512 + w_], True, True, [f'lk{j}', f'Z{q}'], [f'PBb{bk}'])
                            cp('act' if (ce % 3) else 'dve', XhF[:, q, d + hh * 512:d + hh * 512 + w_], PB[:, bk * 512:bk * 512 + w_], [f'PBb{bk}'], [f'Z{q}'])
                for nb in range(4):
                    sl = slice(nb * 512, (nb + 1) * 512)
                    for q in range(8):
                        mm(PA[:, sl], CTp[:, 8 * a + q, :], XhF[:, q, sl], q == 0, q == 7, ['CTp', f'Z{q}'], [f'PAb{nb}'])
                yb_ = a % 2
                ld('sp', y5t[yb_][:], y5_d[a * 128:(a + 1) * 128, :], [], [f'y5t{yb_}'])
                tt('dve', y5t[yb_][:], y5t[yb_][:], PA[:, :], ALU.add, [f'y5t{yb_}'] + [f'PAb{nb}' for nb in range(4)], [f'y5t{yb_}'])
                ld('sp', y5_d[a * 128:(a + 1) * 128, :], y5t[yb_][:], [f'y5t{yb_}'], ['y5_d'])
        P.barrier()
    if stop_after == 'S5':
        P.finish()
        return nc

    with contextlib.ExitStack() as es:
        def SBs(name, shape, dt):
            return es.enter_context(nc.sbuf_tensor(name, shape, dt))
        vT = SBs("vT", [128, 8, TOK], BF16); gT = SBs("gT", [128, 8, TOK], BF16)
        wglu_s = SBs("wglu_s", [128, 8, 1024], BF16); wb_s = SBs("wb_s", [128, 8, 2048], BF16)
        yl = [SBs(f"yl{i}", [128, TOK], F32) for i in range(2)]
        g1 = [SBs(f"g1_{i}", [128, TOK], F32) for i in range(2)]
        sgb = [SBs(f"sgb{i}", [128, 512], BF16) for i in range(2)]
        gbt = [SBs(f"gbt{i}", [128, TOK], BF16) for i in range(2)]
        ybo = [SBs(f"ybo{i}", [128, TOK], BF16) for i in range(2)]
        ld('pool', wglu_s[:], w_glu, [], ['wglu_s'])
        ld('pool', wb_s[:], w_b, [], ['wb_s'])
        for a in range(8):
            b = a % 2
            ld('sp', yl[b][:], y5_d[a * 128:(a + 1) * 128, :], [], [f'yl{b}'])
            act(g1[b][:], yl[b][:], AF.Square, [f'yl{b}'], [f'g1{b}'])
            ts('dve', g1[b][:], g1[b][:], 0.044715, 1.0, ALU.mult, ALU.add, [f'g1{b}'], [f'g1{b}'])
            tt('dve', g1[b][:], g1[b][:], yl[b][:], ALU.mult, [f'g1{b}', f'yl{b}'], [f'g1{b}'])
            act(g1[b][:], g1[b][:], AF.Sigmoid, [f'g1{b}'], [f'g1{b}'], scale=2.0 * math.sqrt(2.0 / math.pi))
            tt('dve', vT[:, a, :], g1[b][:], yl[b][:], ALU.mult, [f'g1{b}', f'yl{b}'], [f'vT{a}'])
        vall = [f'vT{a}' for a in range(8)]
        c = 0
        for bt in range(8):
            for tg in range(4):
                bk = 4 + c % 4
                for a in range(8):
                    mm(bank(bk), wglu_s[:, a, bt * 128:(bt + 1) * 128], vT[:, a, tg * 512:(tg + 1) * 512], a == 0, a == 7,
                       ['wglu_s'] + vall, [f'bank{bk}'])
                sb_ = c % 2
                act(sgb[sb_][:], bank(bk), AF.Sigmoid, [f'bank{bk}'], [f'sgb{sb_}'])
                tt('dve', gT[:, bt, tg * 512:(tg + 1) * 512], vT[:, bt, tg * 512:(tg + 1) * 512], sgb[sb_][:], ALU.mult,
                   [f'sgb{sb_}'] + vall, [f'gT{bt}'])
                c += 1
        gall = [f'gT{a}' for a in range(8)]
        for dt_ in range(16):
            b = dt_ % 2
            ld('sp', gbt[b][:], gs_d[2048 + dt_ * 128: 2048 + (dt_ + 1) * 128, :], [], [f'gbt{b}'])
            for tg in range(4):
                bk = 4 + c % 4
                c += 1
                for a in range(8):
                    mm(bank(bk), wb_s[:, a, dt_ * 128:(dt_ + 1) * 128], gT[:, a, tg * 512:(tg + 1) * 512], a == 0, a == 7,
                       ['wb_s'] + gall, [f'bank{bk}'])
                tt('dve', ybo[b][:, tg * 512:(tg + 1) * 512], bank(bk), gbt[b][:, tg * 512:(tg + 1) * 512], ALU.mult,
                   [f'bank{bk}', f'gbt{b}'], [f'ybo{b}'])
            ld('sp', ybg_d[dt_ * 128:(dt_ + 1) * 128, :], ybo[b][:], [f'ybo{b}'], ['ybg_d'])
        P.barrier()
    if stop_after == 'S5b':
        P.finish()
        return nc

    ssd_stage(True)
    proj_stage()
    if stop_after == 'SSD':
        P.finish()
        return nc

    w12 = SB("w12", [128, NT, 2], F32); desti = SB("desti", [128, NT, 2], I32); idx_all = SB("idx_all", [128, 64], I32)
    tokid_s = SB("tokid_s", [128, NT], I32)
    with contextlib.ExitStack() as es:
        def SBs(name, shape, dt):
            return es.enter_context(nc.sbuf_tensor(name, shape, dt))
        x1l = [SBs(f"x1l{i}", [128, D], F32) for i in range(2)]
        junkM = SBs("junkM", [128, D], BF16)
        h2b = [SBs(f"h2b{i}", [128, D], BF16) for i in range(2)]
        nffn_s = SBs("nffn_s", [128, D], F32)
        h2T = SBs("h2T", [128, 16, 128], BF16)
        wr_s = SBs("wr_s", [128, 16, 36], BF16); br_s = SBs("br_s", [128, 36], F32)
        ecap_s = SBs("ecap_s", [128, 32], F32)
        sinit_s = SBs("sinit_s", [128, 66], I32)
        zrow = SBs("zrow", [128, D], BF16)
        lg = SBs("lg", [128, 36], F32); lm = SBs("lm", [128, 4, 8], F32)
        sc = SBs("sc", [128, 16], F32); mg = SBs("mg", [128, 4], F32); ge = SBs("ge", [128, 4], F32); mx8 = SBs("mx8", [128, 8], F32)
        ssM = SBs("ssM", [128, 4], F32)
        M1f = SBs("M1f", [128, NT, 32], F32); M2f = SBs("M2f", [128, NT, 32], F32); M12b = SBs("M12b", [128, NT, 32], BF16)
        pos = SBs("pos", [128, 32], F32); ovm = SBs("ovm", [128, 32], F32); ptmp = SBs("ptmp", [128, 32], F32)
        destf = SBs("destf", [128, NT, 2], F32)
        ld('sp', nffn_s[:], nffn, [], ['nffn_s']); ld('pool', wr_s[:], w_r, [], ['wr_s']); ld('sp', br_s[:], b_r, [], ['br_s'])
        ld('sp', ecap_s[:], c_ecap, [], ['ecap_s']); ld('sp', tokid_s[:], c_tokid, [], ['tokid_s']); ld('sp', sinit_s[:], c_sinit, [], ['sinit_s'])
        memset('pool', zrow[:], 0.0, ['zrow'])
        ld('sp', h2_d[TOK:TOK + 128, :], zrow[:], ['zrow'], ['h2_dz'])
        ld('sp', yall_d[NSLOT:NSLOT + 128, :], zrow[:], ['zrow'], ['yall_dz'])
        ld('sp', slot_d.rearrange("(p b) o -> p (b o)", p=128), sinit_s[:, 0:65], ['sinit_s'], ['slot_init'])
        for ti in range(NT):
            b = ti % 2
            r0 = ti * 128
            ld('sp', x1l[b][:], x1_d[r0:r0 + 128, :], [], [f'x1l{b}'])
            act(junkM[:], x1l[b][:], AF.Square, [f'x1l{b}'], ['junkM', 'ssM0'], accum=ssM[:, 0:1])
            rstd_from_ss(ssM, ['ssM0'], 'ssMr')
            stt(h2b[b][:], x1l[b][:], ssM[:, 3:4], nffn_s[:], ALU.mult, ALU.mult, [f'x1l{b}', 'ssMr', 'nffn_s'], [f'h2b{b}'])
            ld('sp', h2_d[r0:r0 + 128, :], h2b[b][:], [f'h2b{b}'], [f'h2_d{ti}'])
            for half in range(2):
                pb = bank(half).bitcast(BF16)
                for k in range(8):
                    kt = half * 8 + k
                    tr(pb[:, k * 128:(k + 1) * 128], h2b[b][:, kt * 128:(kt + 1) * 128], idb[:], [f'h2b{b}', 'idb'], [f'bank{half}'])
                cp('act' if half else 'dve', h2T[:, half * 8:(half + 1) * 8, :], pb[:, 0:1024].rearrange("p (a q) -> p a q", a=8), [f'bank{half}'], [f'h2T{half}'])
            for kt in range(16):
                mm(bank(2)[:, 0:36], h2T[:, kt, :], wr_s[:, kt, :], kt == 0, kt == 15, ['h2T0', 'h2T1', 'wr_s'], ['bank2'])
            tt('dve', lg[:], bank(2)[:, 0:36], br_s[:], ALU.add, ['bank2', 'br_s'], ['lg'])
            P.op('dve', lambda e: e.reduce_max(out=sc[:, 0:1], in_=lg[:, 0:4], axis=AX.X), ['lg'], ['sc0'])
            ts('dve', mg[:], lg[:, 0:4], sc[:, 0:1], None, ALU.is_equal, None, ['lg', 'sc0'], ['mg'])
            ts('dve', sc[:, 1:2], sc[:, 0:1], -1.0, None, ALU.mult, None, ['sc0'], ['sc1'])
            act(ge[:], lg[:, 0:4], AF.Exp, ['lg', 'sc1'], ['ge', 'sc2'], bias=sc[:, 1:2], accum=sc[:, 2:3])
            P.op('dve', lambda e: e.reciprocal(out=sc[:, 3:4], in_=sc[:, 2:3]), ['sc2'], ['sc3'])
            ts('dve', mg[:], mg[:], -1.0, 1e9, ALU.add, ALU.mult, ['mg'], ['mg'])
            tt('dve', lm[:], lg[:, 4:36].rearrange("p (a q) -> p a q", a=4), mg[:].unsqueeze(2).to_broadcast([128, 4, 8]), ALU.add, ['lg', 'mg'], ['lm'])
            lm2 = lm[:].rearrange("p a q -> p (a q)")
            P.op('dve', lambda e: e.max(out=mx8[:], in_=lm2), ['lm'], ['mx8'])
            ts('dve', M1f[:, ti, :], lm2, mx8[:, 0:1], None, ALU.is_equal, None, ['lm', 'mx8'], ['M1f'])
            ts('dve', M2f[:, ti, :], lm2, mx8[:, 1:2], None, ALU.is_equal, None, ['lm', 'mx8'], ['M2f'])
            tt('dve', M12b[:, ti, :], M1f[:, ti, :], M2f[:, ti, :], ALU.add, ['M1f', 'M2f'], ['M12b'])
            tt('dve', sc[:, 4:5], mx8[:, 0:1], mx8[:, 1:2], ALU.subtract, ['mx8'], ['sc4'])
            act(sc[:, 5:6], sc[:, 4:5], AF.Sigmoid, ['sc4'], ['sc5'])
            tt('dve', w12[:, ti, 0:1], sc[:, 5:6], sc[:, 3:4], ALU.mult, ['sc5', 'sc3'], ['w12'])
            tt('dve', w12[:, ti, 1:2], sc[:, 3:4], w12[:, ti, 0:1], ALU.subtract, ['sc3', 'w12'], ['w12'])
        for ti in range(NT):
            for t2 in range(ti):
                mm(bank(3)[:, 0:32], onesb[:], M12b[:, t2, :], t2 == 0, False, ['onesb', 'M12b'], ['bank3'])
            mm(bank(3)[:, 0:32], sutb[:], M12b[:, ti, :], ti == 0, True, ['sutb', 'M12b'], ['bank3'])
            ts('dve', ovm[:], bank(3)[:, 0:32], CAP - 0.5, 1e6, ALU.is_ge, ALU.mult, ['bank3'], ['ovm'])
            tt('dve', pos[:], bank(3)[:, 0:32], ecap_s[:], ALU.add, ['bank3', 'ecap_s'], ['pos'])
            tt('dve', pos[:], pos[:], ovm[:], ALU.add, ['pos', 'ovm'], ['pos'])
            ts('dve', pos[:], pos[:], float(NSLOT), None, ALU.min, None, ['pos'], ['pos'])
            for k, Mk in enumerate((M1f, M2f)):
                tt('dve', ptmp[:], Mk[:, ti, :], pos[:], ALU.mult, ['pos', 'M1f', 'M2f'], ['ptmp'])
                P.op('dve', lambda e, k=k, ti=ti: e.reduce_sum(out=destf[:, ti, k:k + 1], in_=ptmp[:], axis=AX.X), ['ptmp'], ['destf'])
        cp('dve', desti[:], destf[:], ['destf'], ['desti'])
        sw = []
        for ti in range(NT):
            for k in range(2):
                nm = f'slot_w{ti}_{k}'
                sw.append(nm)
                P.dma('pool', lambda e, ti=ti, k=k: e.indirect_dma_start(
                    out=slot_d[:, :], out_offset=bass.IndirectOffsetOnAxis(ap=desti[:, ti, k:k + 1], axis=0),
                    in_=tokid_s[:, ti:ti + 1], in_offset=None, bounds_check=NSLOT + 127, oob_is_err=False),
                    ['slot_init', 'desti', 'tokid_s'], [nm])
        for bq in range(64):
            ld('sp', idx_all[:, bq:bq + 1], slot_d[bq * 128:(bq + 1) * 128, :], sw, [f'idx_all{bq}'])
        if 'idx' in dbg_t:
            cp('dve', destf[:].rearrange("p a b -> p (a b)"), desti[:].rearrange("p a b -> p (a b)"), ['desti'], ['destf'])
            ld('sp', dbg_t['idx'][:, 0:32], destf[:].rearrange("p a b -> p (a b)"), ['destf'], ['dbgidx'])
            ld('sp', dbg_t['idx'][:, 32:64], w12[:].rearrange("p a b -> p (a b)"), ['w12'], ['dbgidx2'])
        P.barrier(include_bg=True)
    if stop_after == 'MOEA':
        P.finish()
        return nc

    with contextlib.ExitStack() as es:
        def SBs(name, shape, dt):
            return es.enter_context(nc.sbuf_tensor(name, shape, dt))
        weg = [SBs(f"weg{i}", [128, 16, 512], BF16) for i in range(2)]
        weu = [SBs(f"weu{i}", [128, 16, 512], BF16) for i in range(2)]
        wed = [SBs(f"wed{i}", [128, 4, 2048], BF16) for i in range(2)]
        Xg = [SBs(f"Xg{i}", [128, 2, D], BF16) for i in range(2)]
        XT = SBs("XT", [128, 16, 256], BF16)
        sgs = [SBs(f"sgs{i}", [128, 256], F32) for i in range(2)]
        aT = SBs("aT", [128, 4, 256], BF16)
        Yb = [SBs(f"Yb{i}", [128, D], BF16) for i in range(2)]
        yc = 0
        for ex in range(32):
            j = ex % 2
            ld('sp', weg[j][:], weg_d[ex].rearrange("(p k) n -> p k n", k=16), [], [f'weg{j}'])
            ld('sp', weu[j][:], weu_d[ex].rearrange("(p k) n -> p k n", k=16), [], [f'weu{j}'])
            ld('sp', wed[j][:], wed_d[ex].rearrange("(p k) n -> p k n", k=4), [], [f'wed{j}'])
            for sb in range(2):
                blk = ex * 2 + sb
                P.dma('pool', lambda e, j=j, sb=sb, blk=blk: e.indirect_dma_start(
                    out=Xg[j][:, sb, :], out_offset=None, in_=h2_d[:, :],
                    in_offset=bass.IndirectOffsetOnAxis(ap=idx_all[:, blk:blk + 1], axis=0)),
                    [], [f'Xg{j}_{sb}'])
            for sb in range(2):
                for half in range(2):
                    bk = (sb * 2 + half) % 2
                    pb = bank(bk).bitcast(BF16)
                    for k in range(8):
                        kt = half * 8 + k
                        tr(pb[:, k * 128:(k + 1) * 128], Xg[j][:, sb, kt * 128:(kt + 1) * 128], idb[:], [f'Xg{j}_{sb}', 'idb'], [f'bank{bk}'])
                    cp('act' if half else 'dve', XT[:, half * 8:(half + 1) * 8, sb * 128:(sb + 1) * 128],
                       pb[:, 0:1024].rearrange("p (a q) -> p a q", a=8), [f'bank{bk}'], [f'XT{sb}{half}'])
            XTs = ['XT00', 'XT01', 'XT10', 'XT11']
            for f in range(4):
                ba, bu_ = 2 + f % 2, 4 + f % 2
                for kt in range(16):
                    mm(bank(ba)[:, 0:256], weg[j][:, kt, f * 128:(f + 1) * 128], XT[:, kt, :], kt == 0, kt == 15, [f'weg{j}'] + XTs, [f'bank{ba}'])
                for kt in range(16):
                    mm(bank(bu_)[:, 0:256], weu[j][:, kt, f * 128:(f + 1) * 128], XT[:, kt, :], kt == 0, kt == 15, [f'weu{j}'] + XTs, [f'bank{bu_}'])
                act(sgs[f % 2][:], bank(ba)[:, 0:256], AF.Silu, [f'bank{ba}'], [f'sgs{f % 2}'])
                tt('dve', aT[:, f, :], sgs[f % 2][:], bank(bu_)[:, 0:256], ALU.mult, [f'sgs{f % 2}', f'bank{bu_}'], [f'aT{f}'])
            aTs = [f'aT{f}' for f in range(4)]
            for sb in range(2):
                yb_ = yc % 2
                yc += 1
                for cg in range(4):
                    bk = 6 + cg % 2
                    for f in range(4):
                        mm(bank(bk), aT[:, f, sb * 128:(sb + 1) * 128], wed[j][:, f, cg * 512:(cg + 1) * 512], f == 0, f == 3, [f'wed{j}'] + aTs, [f'bank{bk}'])
                    cp('act' if cg % 2 else 'dve', Yb[yb_][:, cg * 512:(cg + 1) * 512], bank(bk), [f'bank{bk}'], [f'Yb{yb_}'])
                rr = ex * CAP + sb * 128
                ld('sp', yall_d[rr:rr + 128, :], Yb[yb_][:], [f'Yb{yb_}'], ['yall_d'])
        P.barrier()

    with contextlib.ExitStack() as es:
        def SBs(name, shape, dt):
            return es.enter_context(nc.sbuf_tensor(name, shape, dt))
        Y1 = [SBs(f"Y1_{i}", [128, D], BF16) for i in range(2)]
        Y2 = [SBs(f"Y2_{i}", [128, D], BF16) for i in range(2)]
        xf = [SBs(f"xf{i}", [128, D], F32) for i in range(2)]
        of = [SBs(f"of{i}", [128, D], F32) for i in range(2)]
        junkF = SBs("junkF", [128, D], BF16)
        nfin_s = SBs("nfin_s", [128, D], F32)
        ssF = SBs("ssF", [128, 4], F32)
        ld('sp', nfin_s[:], nfin, [], ['nfin_s'])
        for ti in range(NT):
            b = ti % 2
            r0 = ti * 128
            for k, Yk in enumerate((Y1, Y2)):
                P.dma('pool', lambda e, Yk=Yk, b=b, ti=ti, k=k: e.indirect_dma_start(
                    out=Yk[b][:], out_offset=None, in_=yall_d[:, :],
                    in_offset=bass.IndirectOffsetOnAxis(ap=desti[:, ti, k:k + 1], axis=0)),
                    [], [f'Y{k}_{b}'])
            ld('sp', xf[b][:], x1_d[r0:r0 + 128, :], [], [f'xf{b}'])
            stt(xf[b][:], Y1[b][:], w12[:, ti, 0:1], xf[b][:], ALU.mult, ALU.add, [f'Y0_{b}', f'xf{b}'], [f'xf{b}'])
            stt(xf[b][:], Y2[b][:], w12[:, ti, 1:2], xf[b][:], ALU.mult, ALU.add, [f'Y1_{b}', f'xf{b}'], [f'xf{b}'])
            act(junkF[:], xf[b][:], AF.Square, [f'xf{b}'], ['junkF', 'ssF0'], accum=ssF[:, 0:1])
            rstd_from_ss(ssF, ['ssF0'], 'ssFr')
            stt(of[b][:], xf[b][:], ssF[:, 3:4], nfin_s[:], ALU.mult, ALU.mult, [f'xf{b}', 'ssFr', 'nfin_s'], [f'of{b}'])
            ld('sp', out[r0:r0 + 128, :], of[b][:], [f'of{b}'], ['out'])

    P.finish()
    return nc


def host_inputs(inp, c):
    f = np.float32
    x = np.asarray(inp["x"], f)[0]
    t0 = c * TOK
    m = {}
    m["x_c"] = np.ascontiguousarray(x[t0:t0 + TOK])
    xh = np.zeros((128, D), f)
    if c > 0:
        xh[125:128] = x[t0 - 3:t0]
    m["x_h"] = xh
    w_in = np.asarray(inp["w_in"], f)[0]
    m["w_z"] = ktl(w_in[:, 0:2048]); m["w_xbc"] = ktl(w_in[:, 2048:6144]); m["w_dt"] = ktl(w_in[:, 6144:6176])
    m["w_u"] = ktl(w_in[:, 6176:7200]); m["w_g"] = ktl(w_in[:, 7200:11296])
    m["nwmix"] = col(np.asarray(inp["norm_mix_w"], f)[0])
    cwv = np.asarray(inp["conv_w"], f)[0]
    m["cw"] = np.ascontiguousarray(cwv.T.reshape(32, 128, 4).transpose(1, 0, 2))
    m["cb"] = col(np.asarray(inp["conv_b"], f)[0])
    m["dtb"] = rep(np.asarray(inp["dt_bias"], f)[0]); m["alog"] = rep(np.asarray(inp["a_log"], f)[0])
    m["dssd"] = rep(np.asarray(inp["d_ssd"], f)[0])
    m["nssd"] = col(np.asarray(inp["norm_ssd_w"], f)[0]); m["gateb"] = col(np.asarray(inp["gate_b"], f)[0])
    m["w_a"] = ktl(np.asarray(inp["w_a_up"], f)[0]); m["w_o"] = ktl(np.asarray(inp["w_out"], f)[0])
    lr = np.asarray(inp["s5_lambda_re"], f)[0]; li = np.asarray(inp["s5_lambda_im"], f)[0]
    m["lre"] = np.ascontiguousarray(np.concatenate([lr.T, lr.T], 0)); m["lim"] = np.ascontiguousarray(np.concatenate([li.T, li.T], 0))
    m["ldt"] = rep(np.asarray(inp["s5_log_dt"], f)[0])
    bre = np.asarray(inp["s5_b_re"], f)[0].transpose(1, 0, 2); bim = np.asarray(inp["s5_b_im"], f)[0].transpose(1, 0, 2)
    m["b1"] = np.ascontiguousarray(np.concatenate([bre, bim], 0)); m["b2"] = np.ascontiguousarray(np.concatenate([bim, bre], 0))
    cre = np.asarray(inp["s5_c_re"], f)[0].transpose(2, 0, 1); cim = np.asarray(inp["s5_c_im"], f)[0].transpose(2, 0, 1)
    m["c1"] = np.ascontiguousarray(np.concatenate([cre, cim], 0))
    m["d5"] = col(np.asarray(inp["s5_d"], f)[0])
    m["w_glu"] = ktl(np.asarray(inp["w_glu"], f)[0]); m["w_b"] = ktl(np.asarray(inp["w_b_up"], f)[0])
    m["nffn"] = rep(np.asarray(inp["norm_ffn_w"], f)[0]); m["nfin"] = rep(np.asarray(inp["norm_final_w"], f))
    wr = np.concatenate([np.asarray(inp["w_route_group"], f)[0], np.asarray(inp["w_route_expert"], f)[0]], 1)
    m["w_r"] = ktl(wr)
    m["b_r"] = rep(np.concatenate([np.asarray(inp["b_route_group"], f)[0], np.asarray(inp["b_route_expert"], f)[0]]))
    eg = np.asarray(inp["w_exp_gate"], f)[0]; eu = np.asarray(inp["w_exp_up"], f)[0]; ed = np.asarray(inp["w_exp_down"], f)[0]
    m["w_eg"] = np.ascontiguousarray(eg.reshape(32, 16, 128, 512).transpose(0, 2, 1, 3))
    m["w_eu"] = np.ascontiguousarray(eu.reshape(32, 16, 128, 512).transpose(0, 2, 1, 3))
    m["w_ed"] = np.ascontiguousarray(ed.reshape(32, 4, 128, 2048).transpose(0, 2, 1, 3))
    ar = np.arange(128)
    m["c_id"] = np.eye(128, dtype=f)
    m["c_ut"] = (ar[:, None] <= ar[None, :]).astype(f)
    m["c_gt"] = (ar[:, None] > ar[None, :]).astype(f)
    m["c_sut"] = (ar[:, None] < ar[None, :]).astype(f)
    psw = np.zeros((128, 128), f); psw[ar, (ar + 64) % 128] = 1
    m["c_psw"] = psw
    sg = np.ones((128, 2), f); sg[64:, 0] = -1; sg[:64, 1] = -1
    m["c_sign"] = sg
    m["c_mcol"] = (ar[:, None] // 16 == np.arange(8)[None, :]).astype(f)
    m["c_ecap"] = rep((np.arange(32) * CAP).astype(f))
    m["c_tokid"] = (np.arange(16)[None, :] * 128 + ar[:, None]).astype(np.int32)
    m["c_sinit"] = np.full((128, 66), TOK, np.int32)
    m["c_mk"] = rep((np.arange(8) < c).astype(f))
    return m


_CACHE = {}


def kernel(**inputs):
    if "nc" not in _CACHE:
        _CACHE["nc"] = build(ncores=NCORES)
    nc = _CACHE["nc"]
    in_maps = [host_inputs(inputs, c) for c in range(NCORES)]
    res = run_bass_kernel_spmd(nc, in_maps, core_ids=list(range(NCORES)))
    outs = [np.asarray(r["out"], np.float32) for r in res.results]
    return np.concatenate(outs, 0).reshape(1, NCORES * TOK, D)
```
